# Optimizing a Trainium2 kernel written in Bass

```python
import math
import jax, jax.numpy as jnp
from jax import lax
import numpy as np

D_MODEL = 2048
BATCH = 4
SEQ = 4096
DEPTH = 1
DEC_BATCH = 16
DEC_SEQ = 2048
PAST_LEN = 128

HEAD_DIM = 64
ATTN_WIDTH = D_MODEL // 2
ATTN_HEADS = ATTN_WIDTH // HEAD_DIM
KV_HEADS = ATTN_HEADS // 4
KV_WIDTH = KV_HEADS * HEAD_DIM
WINDOW = 128
ATTN_BLOCK = 128
REL_BUCKETS = 32
REL_MAX_DIST = 128
NEG_INF = -1e30
RWKV_WIDTH = D_MODEL - ATTN_WIDTH
RWKV_HEAD = 64
RWKV_HEADS = RWKV_WIDTH // RWKV_HEAD
DECAY_LORA = 64
ICLR_LORA = 64
GATE_LORA = 160
GN_EPS = 64e-5
RWKV_COLS = 3 * RWKV_WIDTH + GATE_LORA + 2 * DECAY_LORA + 2 * ICLR_LORA
N_IN_COLS = ATTN_WIDTH + 2 * KV_WIDTH + RWKV_COLS
N_GROUPS = 8
EXPERTS_PER_GROUP = 8
N_EXPERTS = N_GROUPS * EXPERTS_PER_GROUP
TOP_K = 2
D_EXPERT = 512
MOE_BLOCK = 128
LN_EPS = 1e-5

kernel_name = "hymba_swa_rwkv7_hmoe_encoder"


def layer_norm(x, g, b):
    xf = x.astype(jnp.float32)
    mu = jnp.mean(xf, -1, keepdims=True)
    var = jnp.mean(jnp.square(xf - mu), -1, keepdims=True)
    return ((xf - mu) * lax.rsqrt(var + LN_EPS) * g + b).astype(x.dtype)


def t5_bucket(rel):
    half = REL_BUCKETS // 2
    max_exact = half // 2
    ret = jnp.where(rel > 0, half, 0)
    n = jnp.abs(rel)
    nf = jnp.maximum(n, 1).astype(jnp.float32)
    large = max_exact + (jnp.log(nf / max_exact) / math.log(REL_MAX_DIST / max_exact)
                         * (half - max_exact)).astype(jnp.int32)
    large = jnp.minimum(large, half - 1)
    return ret + jnp.where(n < max_exact, n, large)


def windowed_sink_attention(q, k, v, sink, rel_bias):
    B, S = q.shape[0], q.shape[1]
    nb = S // ATTN_BLOCK
    G = ATTN_HEADS // KV_HEADS
    qb = q.reshape(B, nb, ATTN_BLOCK, KV_HEADS, G, HEAD_DIM)

    def band(t):
        tp = jnp.pad(t, ((0, 0), (ATTN_BLOCK, ATTN_BLOCK), (0, 0), (0, 0)))
        tp = tp.reshape(B, nb + 2, ATTN_BLOCK, KV_HEADS, HEAD_DIM)
        return jnp.concatenate([tp[:, :-2], tp[:, 1:-1], tp[:, 2:]], axis=2)

    kw, vw = band(k), band(v)
    qi = jnp.arange(ATTN_BLOCK)[:, None]
    kj = jnp.arange(3 * ATTN_BLOCK)[None, :]
    rel = kj - ATTN_BLOCK - qi
    bias = rel_bias[t5_bucket(rel)].astype(jnp.float32)
    bias = jnp.transpose(bias, (2, 0, 1)).reshape(KV_HEADS, G, ATTN_BLOCK, 3 * ATTN_BLOCK)
    kpos = jnp.arange(nb)[:, None, None] * ATTN_BLOCK - ATTN_BLOCK + kj[None]
    mask = (jnp.abs(rel) <= WINDOW)[None] & (kpos >= 0) & (kpos < S)
    scores = jnp.einsum('bnqhgd,bnkhd->bnhgqk', qb, kw,
                        preferred_element_type=jnp.float32) * (HEAD_DIM ** -0.5)
    logits = jnp.where(mask[None, :, None, None], scores + bias, NEG_INF)
    sink_l = sink.astype(jnp.float32).reshape(KV_HEADS, G, 1, 1)
    m = jnp.maximum(jnp.max(logits, -1, keepdims=True), sink_l)
    p = jnp.exp(logits - m)
    denom = jnp.sum(p, -1, keepdims=True) + jnp.exp(sink_l - m)
    probs = (p / denom).astype(v.dtype)
    out = jnp.einsum('bnhgqk,bnkhd->bnqhgd', probs, vw)
    return out.reshape(B, S, ATTN_WIDTH)


def centred_shift(p, mu):
    zeros = jnp.zeros_like(p[:, :1])
    prev = jnp.concatenate([zeros, p[:, :-1]], axis=1)
    nxt = jnp.concatenate([p[:, 1:], zeros], axis=1)
    return p + mu * (0.5 * (prev + nxt) - p)


def rwkv7_bidirectional(p, mu, w0, w2, a0, a2, g2, k_k, k_a, r_k, gn_g, gn_b):
    B, S = p.shape[0], p.shape[1]
    f32 = jnp.float32
    RW = RWKV_WIDTH
    p = centred_shift(p, mu)
    r, k, v, gd, wd, ad = jnp.split(
        p, [RW, 2 * RW, 3 * RW, 3 * RW + GATE_LORA, 3 * RW + GATE_LORA + 2 * DECAY_LORA], axis=-1)
    wd = wd.reshape(B, S, 2, DECAY_LORA)
    ad = ad.reshape(B, S, 2, ICLR_LORA)
    wlog = (w0 + jnp.einsum('bsdr,drc->bsdc', jnp.tanh(wd), w2)).astype(f32)
    decay = jnp.exp(-jnp.exp(-jax.nn.softplus(-wlog) - 0.5))
    a = jax.nn.sigmoid((a0 + jnp.einsum('bsdr,drc->bsdc', ad, a2)).astype(f32))
    g = jnp.einsum('bsr,rc->bsc', jax.nn.sigmoid(gd), g2).astype(f32)
    rf, kf, vf = r.astype(f32), k.astype(f32), v.astype(f32)
    kd = kf[:, :, None] * (1.0 + (a - 1.0) * k_a)

    def heads(t):
        return t.reshape(t.shape[:-1] + (RWKV_HEADS, RWKV_HEAD))

    kk = heads(kf * k_k)
    kk = kk / jnp.maximum(jnp.sqrt(jnp.sum(kk * kk, -1, keepdims=True)), 1e-12)
    r_h, v_h = heads(rf), heads(vf)
    kd_h, a_h, w_h = heads(kd), heads(a), heads(decay)
    dir_shape = kd_h.shape

    def to_scan(t):
        t = jnp.stack([t[:, :, 0], jnp.flip(t[:, :, 1], axis=1)], axis=0)
        return jnp.transpose(t, (2, 0, 1, 3, 4))

    def both(t):
        return jnp.broadcast_to(t[:, :, None], dir_shape)

    def step(state, inp):
        r_t, w_t, k_t, v_t, a_t, b_t = inp
        sa = jnp.einsum('dbhvk,dbhk->dbhv', state, a_t)
        state = (state * w_t[..., None, :] + sa[..., :, None] * b_t[..., None, :]
                 + v_t[..., :, None] * k_t[..., None, :])
        y = jnp.einsum('dbhvk,dbhk->dbhv', state, r_t)
        return state, y

    state0 = jnp.zeros((2, B, RWKV_HEADS, RWKV_HEAD, RWKV_HEAD), f32)
    xs = (to_scan(both(r_h)), to_scan(w_h), to_scan(kd_h), to_scan(both(v_h)),
          to_scan(both(-kk)), to_scan(both(kk)[...] * a_h))
    _, ys = lax.scan(step, state0, xs)
    ys = jnp.transpose(ys, (1, 2, 0, 3, 4))
    y = ys[0] + jnp.flip(ys[1], axis=1)
    bonus = jnp.sum(jnp.sum(r_h[:, :, None] * kd_h * r_k, -1, keepdims=True) * v_h[:, :, None], axis=2)
    y = y + bonus
    ym = jnp.mean(y, -1, keepdims=True)
    yv = jnp.mean(jnp.square(y - ym), -1, keepdims=True)
    y = ((y - ym) * lax.rsqrt(yv + GN_EPS)).reshape(B, S, RW) * gn_g + gn_b
    return (y * g).astype(p.dtype)


def hierarchical_moe(h, wr_group, br_group, wr_expert, br_expert, w_gate, w_up, w_down):
    B, S, D = h.shape
    T = B * S
    f32 = jnp.float32
    x = h.reshape(T, D)
    g_logits = (x @ wr_group).astype(f32) + br_group
    g_prob = jax.nn.softmax(g_logits, axis=-1)
    g_idx = jnp.argmax(g_logits, axis=-1).astype(jnp.int32)
    g_gate = jnp.take_along_axis(g_prob, g_idx[:, None], axis=-1)
    e_logits = ((x @ wr_expert).astype(f32) + br_expert).reshape(T, N_GROUPS, EXPERTS_PER_GROUP)
    e_logits = jnp.take_along_axis(e_logits, g_idx[:, None, None], axis=1)[:, 0]
    top_l, top_i = lax.top_k(e_logits, TOP_K)
    gates = g_gate * jax.nn.softmax(top_l, axis=-1)
    expert_id = g_idx[:, None] * EXPERTS_PER_GROUP + top_i.astype(jnp.int32)

    N = T * TOP_K
    flat_e = expert_id.reshape(N)
    flat_tok = jnp.repeat(jnp.arange(T, dtype=jnp.int32), TOP_K)
    flat_w = gates.reshape(N)
    order = jnp.argsort(flat_e)
    sorted_e = flat_e[order]
    counts = jnp.bincount(flat_e, length=N_EXPERTS).astype(jnp.int32)
    start = jnp.cumsum(counts) - counts
    padded = (counts + MOE_BLOCK - 1) // MOE_BLOCK * MOE_BLOCK
    pad_end = jnp.cumsum(padded)
    pad_start = pad_end - padded
    dest = pad_start[sorted_e] + jnp.arange(N, dtype=jnp.int32) - start[sorted_e]
    n_blocks = -(-N // MOE_BLOCK) + N_EXPERTS
    R = n_blocks * MOE_BLOCK
    row_tok = jnp.full((R,), T, jnp.int32).at[dest].set(flat_tok[order])
    row_w = jnp.zeros((R,), f32).at[dest].set(flat_w[order])
    blk_start = jnp.arange(n_blocks, dtype=jnp.int32) * MOE_BLOCK
    blk_e = jnp.minimum(jnp.searchsorted(pad_end, blk_start, side='right'), N_EXPERTS - 1)
    x_pad = jnp.concatenate([x, jnp.zeros((1, D), x.dtype)], axis=0)
    xb = x_pad[row_tok].reshape(n_blocks, MOE_BLOCK, D)

    def expert_block(args):
        xe, e = args
        hid = jax.nn.silu(xe @ w_gate[e]) * (xe @ w_up[e])
        return hid @ w_down[e]

    yb = lax.map(expert_block, (xb, blk_e)).reshape(R, D)
    y = jax.ops.segment_sum(yb * row_w[:, None].astype(yb.dtype), row_tok, num_segments=T + 1)[:T]
    return y.reshape(B, S, D).astype(h.dtype)


def encoder_layer(x, rel_bias, w_in, attn_sink, shift_mu, decay_w0, decay_w2, iclr_a0, iclr_a2,
                  gate_g2, k_k, k_a, r_k, gn_g, gn_b, w_out, ln1_g, ln1_b,
                  router_group_w, router_group_b, router_expert_w, router_expert_b,
                  w_gate, w_up, w_down, ln2_g, ln2_b):
    alpha = (2.0 * DEPTH) ** 0.25
    B, S, _ = x.shape
    proj = x @ w_in
    q, k, v, p_rwkv = jnp.split(proj, [ATTN_WIDTH, ATTN_WIDTH + KV_WIDTH, ATTN_WIDTH + 2 * KV_WIDTH], axis=-1)
    q = q.reshape(B, S, ATTN_HEADS, HEAD_DIM)
    k = k.reshape(B, S, KV_HEADS, HEAD_DIM)
    v = v.reshape(B, S, KV_HEADS, HEAD_DIM)
    attn = windowed_sink_attention(q, k, v, attn_sink, rel_bias)
    rw = rwkv7_bidirectional(p_rwkv, shift_mu, decay_w0, decay_w2, iclr_a0, iclr_a2, gate_g2,
                             k_k, k_a, r_k, gn_g, gn_b)
    mix = jnp.concatenate([attn, rw], axis=-1) @ w_out
    h = layer_norm(alpha * x + mix, ln1_g, ln1_b)
    ffn = hierarchical_moe(h, router_group_w, router_group_b, router_expert_w, router_expert_b,
                           w_gate, w_up, w_down)
    return layer_norm(alpha * h + ffn, ln2_g, ln2_b)


def encoder_trunk(x, rel_bias, w_in, attn_sink, shift_mu, decay_w0, decay_w2, iclr_a0, iclr_a2,
                  gate_g2, k_k, k_a, r_k, gn_g, gn_b, w_out, ln1_g, ln1_b,
                  router_group_w, router_group_b, router_expert_w, router_expert_b,
                  w_gate, w_up, w_down, ln2_g, ln2_b):
    for l in range(DEPTH):
        x = encoder_layer(x, rel_bias, w_in[l], attn_sink[l], shift_mu[l], decay_w0[l], decay_w2[l],
                          iclr_a0[l], iclr_a2[l], gate_g2[l], k_k[l], k_a[l], r_k[l], gn_g[l], gn_b[l],
                          w_out[l], ln1_g[l], ln1_b[l], router_group_w[l], router_group_b[l],
                          router_expert_w[l], router_expert_b[l], w_gate[l], w_up[l], w_down[l],
                          ln2_g[l], ln2_b[l])
    return x


def setup_inputs(seed: int = 0) -> dict:
    key = jax.random.key(seed)
    ks = jax.random.split(key, 32)
    f32 = jnp.float32
    beta = (8.0 * DEPTH) ** -0.25
    D, L, RW = D_MODEL, DEPTH, RWKV_WIDTH

    def nrm(k, shape, s):
        return jax.random.normal(k, shape, f32) * s

    col_scale = jnp.concatenate([
        jnp.ones((ATTN_WIDTH + KV_WIDTH,), f32), jnp.full((KV_WIDTH,), beta, f32),
        jnp.ones((2 * RW,), f32), jnp.full((RW,), beta, f32),
        jnp.ones((GATE_LORA + 2 * DECAY_LORA + 2 * ICLR_LORA,), f32)])
    return {
        "x_prompt": nrm(ks[0], (BATCH, SEQ, D), 1.0),
        "x_sample": nrm(ks[1], (DEC_BATCH, DEC_SEQ, D), 1.0),
        "rel_bias": nrm(ks[2], (REL_BUCKETS, ATTN_HEADS), 0.5),
        "w_in": nrm(ks[3], (L, D, N_IN_COLS), D ** -0.5) * col_scale,
        "attn_sink": nrm(ks[4], (L, ATTN_HEADS), 0.5),
        "shift_mu": jax.random.uniform(ks[5], (L, RWKV_COLS), f32, 0.1, 0.9),
        "decay_w0": jax.random.uniform(ks[6], (L, 2, RW), f32, -6.0, 1.0),
        "decay_w2": nrm(ks[7], (L, 2, DECAY_LORA, RW), 0.5 * DECAY_LORA ** -0.5),
        "iclr_a0": nrm(ks[8], (L, 2, RW), 0.1),
        "iclr_a2": nrm(ks[9], (L, 2, ICLR_LORA, RW), 0.5 * ICLR_LORA ** -0.5),
        "gate_g2": nrm(ks[10], (L, GATE_LORA, RW), GATE_LORA ** -0.5),
        "k_k": 0.85 + nrm(ks[11], (L, RW), 0.02),
        "k_a": 1.0 + nrm(ks[12], (L, RW), 0.02),
        "r_k": nrm(ks[13], (L, RWKV_HEADS, RWKV_HEAD), 0.1),
        "gn_g": 1.0 + nrm(ks[14], (L, RW), 0.02),
        "gn_b": nrm(ks[15], (L, RW), 0.02),
        "w_out": nrm(ks[16], (L, D, D), beta * D ** -0.5),
        "ln1_g": 1.0 + nrm(ks[17], (L, D), 0.02),
        "ln1_b": nrm(ks[18], (L, D), 0.02),
        "router_group_w": nrm(ks[19], (L, D, N_GROUPS), D ** -0.5),
        "router_group_b": nrm(ks[20], (L, N_GROUPS), 0.01),
        "router_expert_w": nrm(ks[21], (L, D, N_EXPERTS), D ** -0.5),
        "router_expert_b": nrm(ks[22], (L, N_EXPERTS), 0.01),
        "w_gate": nrm(ks[23], (L, N_EXPERTS, D, D_EXPERT), D ** -0.5),
        "w_up": nrm(ks[24], (L, N_EXPERTS, D, D_EXPERT), D ** -0.5),
        "w_down": nrm(ks[25], (L, N_EXPERTS, D_EXPERT, D), beta * D_EXPERT ** -0.5),
        "ln2_g": 1.0 + nrm(ks[26], (L, D), 0.02),
        "ln2_b": nrm(ks[27], (L, D), 0.02),
    }


def reference(x_prompt, x_sample, rel_bias, w_in, attn_sink, shift_mu, decay_w0, decay_w2,
              iclr_a0, iclr_a2, gate_g2, k_k, k_a, r_k, gn_g, gn_b, w_out, ln1_g, ln1_b,
              router_group_w, router_group_b, router_expert_w, router_expert_b,
              w_gate, w_up, w_down, ln2_g, ln2_b):
    y_prompt = encoder_trunk(x_prompt, rel_bias, w_in, attn_sink, shift_mu, decay_w0, decay_w2,
                             iclr_a0, iclr_a2, gate_g2, k_k, k_a, r_k, gn_g, gn_b, w_out, ln1_g, ln1_b,
                             router_group_w, router_group_b, router_expert_w, router_expert_b,
                             w_gate, w_up, w_down, ln2_g, ln2_b)
    y_sample = encoder_trunk(x_sample, rel_bias, w_in, attn_sink, shift_mu, decay_w0, decay_w2,
                             iclr_a0, iclr_a2, gate_g2, k_k, k_a, r_k, gn_g, gn_b, w_out, ln1_g, ln1_b,
                             router_group_w, router_group_b, router_expert_w, router_expert_b,
                             w_gate, w_up, w_down, ln2_g, ln2_b)
    return (y_prompt, y_sample)
```

```python
import contextlib
import math
import numpy as np
import concourse.bass as bass
import concourse.mybir as mybir
from concourse.bass_utils import run_bass_kernel_spmd

F32 = mybir.dt.float32
BF16 = mybir.dt.bfloat16
I32 = mybir.dt.int32
ALU = mybir.AluOpType
AF = mybir.ActivationFunctionType
AX = mybir.AxisListType

EPOCH = 12000
DMA_RING = 8
NEG = -1.0e30
SEMSTORE = {}
SEMBASE = {}


class Buf:
    __slots__ = ("name", "w", "r", "gen", "excl")

    def __init__(self, name=""):
        self.name = name
        self.w = None
        self.r = []
        self.gen = None
        self.excl = False


class Op:
    __slots__ = ("eng", "fn", "dma", "deps", "flag", "ev", "idx")


class Phase:
    ENGS = ("pe", "act", "dve", "pool", "sp")

    def __init__(self, nc, name):
        self.nc = nc
        self.name = name
        self.ops = []

    def op(self, eng, fn, reads=(), writes=(), dma=False):
        o = Op()
        o.eng, o.fn, o.dma, o.flag, o.ev = eng, fn, dma, False, None
        o.idx = len(self.ops)
        for b in list(reads) + list(writes):
            if b.gen is not self:
                b.w, b.r, b.gen = None, [], self
        deps = set()
        xr = [b for b in reads if b.excl]
        if xr:
            reads = [b for b in reads if not b.excl]
            writes = list(writes) + [b for b in xr if b not in writes]
        for b in reads:
            if b.w is not None:
                deps.add(b.w)
        for b in writes:
            if b.w is not None:
                deps.add(b.w)
            for r in b.r:
                deps.add(r)
        deps.discard(o.idx)
        if eng == "pe":
            deps = {d for d in deps if self.ops[d].eng != "pe"}
        o.deps = deps
        for b in reads:
            b.r.append(o.idx)
        for b in writes:
            b.w = o.idx
            b.r = []
        self.ops.append(o)
        return o

    def emit(self):
        nc = self.nc
        ops = self.ops
        for o in ops:
            for d in o.deps:
                ops[d].flag = True
        per = {e: [o for o in ops if o.eng == e] for e in self.ENGS}
        sem_needed = {}
        newbase = {}
        for e in self.ENGS:
            c = 0
            nd = 0
            for o in per[e]:
                if o.dma:
                    base = SEMBASE.get(("d" + e, nd % DMA_RING), 0) if e == "pool" else 0
                    o.ev = ("d" + e, nd % DMA_RING, base + 16 * (nd // DMA_RING + 1), nd)
                    if e == "pool":
                        newbase[("d" + e, nd % DMA_RING)] = o.ev[2]
                    nd += 1
                elif o.flag:
                    o.ev = ("c" + e, c // EPOCH, (c % EPOCH) + 1, None)
                    c += 1
            sem_needed["c" + e] = (c + EPOCH - 1) // EPOCH
            sem_needed["d" + e] = min(nd, DMA_RING)
        with contextlib.ExitStack() as st:
            sems = {}
            for k, n in sem_needed.items():
                for i in range(n):
                    if (k, i) not in SEMSTORE:
                        SEMSTORE[(k, i)] = nc.semaphore(f"g_{k}{i}").__enter__()
                    sems[(k, i)] = SEMSTORE[(k, i)]
            blk = nc.Block()
            blk = blk.__enter__()

            def run(e, eng):
                known = {}
                dma_list = [o for o in per[e] if o.dma]

                def wait(key, val):
                    if known.get(key, 0) >= val:
                        return
                    eng.wait_ge(sems[key], val)
                    known[key] = val

                for o in per[e]:
                    for d in sorted(o.deps):
                        k, i, v, _ = ops[d].ev
                        wait((k, i), v)
                    if o.dma:
                        k, i, v, nd = o.ev
                        if nd >= DMA_RING:
                            wait((k, i), v - 16)
                        o.fn(eng).then_inc(sems[(k, i)], 16)
                    else:
                        ins = o.fn(eng)
                        if o.flag:
                            k, i, v, _ = o.ev
                            ins.then_inc(sems[(k, i)], 1)
                for o in dma_list[-DMA_RING:]:
                    k, i, v, _ = o.ev
                    wait((k, i), v)

            for e, deco in (("pe", blk.tensor), ("act", blk.scalar), ("dve", blk.vector),
                            ("pool", blk.gpsimd), ("sp", blk.sync)):
                if per[e]:
                    deco(lambda eng, e=e: run(e, eng))
            blk.__exit__(None, None, None)
            with nc.Block() as blk2:
                @blk2.sync
                def _(eng):
                    for key_, sm_ in sems.items():
                        if key_[0] != "dpool":
                            eng.sem_clear(sm_)
            SEMBASE.update(newbase)
        self.ops = []


class T:
    def __init__(self, t, nb=1):
        self.t = t
        self.b = [Buf() for _ in range(nb)]

    def __getitem__(self, k):
        return self.t[k]


class Cfg:
    def __init__(self, D=2048, NT=48, NG=8, EPG=8, DE=512, GL=160, DL=64, IL=64):
        self.D = D
        self.KT = D // 128
        self.NT = NT
        self.TPC = NT * 128
        self.AW = D // 2
        self.AH = self.AW // 64
        self.KVH = self.AH // 4
        self.KVW = self.KVH * 64
        self.RW = D - self.AW
        self.RH = self.RW // 64
        self.GL, self.DL, self.IL = GL, DL, IL
        self.RC = 3 * self.RW + GL + 2 * DL + 2 * IL
        self.NIN = self.AW + 2 * self.KVW + self.RC
        self.NG, self.EPG, self.NE, self.DE = NG, EPG, NG * EPG, DE
        self.NB = (self.TPC * 2) // 128 + self.NE
        self.R = self.NB * 128
        self.alpha = 2.0 ** 0.25


class Builder:
    def __init__(self, cfg):
        self.c = cfg
        self.nc = bass.Bass("TRN2", target_bir_lowering=False)
        SEMSTORE.clear()
        SEMBASE.clear()
        self.ph = None
        self.dbg = ()
        self.cut = 0

    def dma(self, eng, out, in_, reads=(), writes=()):
        self.ph.op(eng, lambda e: e.dma_start(out=out, in_=in_), reads, writes, dma=True)

    def mm(self, out, lhsT, rhs, start, stop, reads=(), writes=()):
        self.ph.op("pe", lambda e: e.matmul(out, lhsT=lhsT, rhs=rhs, start=start, stop=stop),
                   reads, writes)

    def tr(self, out, in_, ident, reads=(), writes=()):
        self.ph.op("pe", lambda e: e.transpose(out, in_, ident), reads, writes)

    def act(self, out, in_, func, reads=(), writes=(), bias=None, scale=None, accum=None):
        kw = {}
        if bias is not None:
            kw["bias"] = bias
        if scale is not None:
            kw["scale"] = scale
        if accum is not None:
            kw["accum_out"] = accum
        self.ph.op("act", lambda e: e.activation(out=out, in_=in_, func=func, **kw), reads, writes)

    def tt(self, eng, out, in0, in1, op, reads=(), writes=()):
        self.ph.op(eng, lambda e: e.tensor_tensor(out=out, in0=in0, in1=in1, op=op), reads, writes)

    def ts(self, eng, out, in0, s1, s2, op0, op1=None, reads=(), writes=()):
        if op1 is None:
            self.ph.op(eng, lambda e: e.tensor_scalar(out=out, in0=in0, scalar1=s1, scalar2=None, op0=op0),
                       reads, writes)
        else:
            self.ph.op(eng, lambda e: e.tensor_scalar(out=out, in0=in0, scalar1=s1, scalar2=s2,
                                                      op0=op0, op1=op1), reads, writes)

    def stt(self, eng, out, in0, scalar, in1, op0, op1, reads=(), writes=()):
        self.ph.op(eng, lambda e: e.scalar_tensor_tensor(out=out, in0=in0, scalar=scalar, in1=in1,
                                                         op0=op0, op1=op1), reads, writes)

    def cp(self, eng, out, in_, reads=(), writes=()):
        if eng == "act":
            self.ph.op(eng, lambda e: e.copy(out=out, in_=in_), reads, writes)
        else:
            self.ph.op(eng, lambda e: e.tensor_copy(out=out, in_=in_), reads, writes)

    def red(self, out, in_, op, reads=(), writes=(), negate=False, axis=AX.X):
        self.ph.op("dve", lambda e: e.tensor_reduce(out=out, in_=in_, axis=axis, op=op, negate=negate),
                   reads, writes)

    def memset(self, eng, ap, val, writes=()):
        self.ph.op(eng, lambda e: e.memset(ap, val), (), writes)

    def declare(self):
        c, nc = self.c, self.nc
        I = lambda n, s, d=F32: nc.dram_tensor(n, s, d, kind="ExternalInput").ap()
        self.x = I("x", [c.TPC, c.D])
        self.flags = I("flags", [128, 6, c.NT])
        self.consts = I("consts", [128, 8, 128])
        self.bimat = I("bimat", [128, 2, 384])
        self.consts2 = I("consts2", [128, c.NB + c.KT + c.DE // 128])
        self.rel_bias = I("rel_bias", [32 * c.AH])
        self.w_in = I("w_in", [c.D + 1, c.NIN])
        self.attn_sink = I("attn_sink", [c.AH])
        self.shift_mu = I("shift_mu", [c.RC])
        self.decay_w0 = I("decay_w0", [2, c.RW])
        self.decay_w2 = I("decay_w2", [2, c.DL, c.RW])
        self.iclr_a0 = I("iclr_a0", [2, c.RW])
        self.iclr_a2 = I("iclr_a2", [2, c.IL, c.RW])
        self.gate_g2 = I("gate_g2", [c.GL, c.RW])
        self.k_k = I("k_k", [c.RW])
        self.k_a = I("k_a", [c.RW])
        self.r_k = I("r_k", [c.RW])
        self.gn_g = I("gn_g", [c.RW])
        self.gn_b = I("gn_b", [c.RW])
        self.w_out = I("w_out", [c.D + 1, c.D])
        self.ln1_g = I("ln1_g", [c.D])
        self.ln1_b = I("ln1_b", [c.D])
        self.wr = I("wr", [c.D, c.NG + c.NE])
        self.br = I("br", [c.NG + c.NE])
        self.w_gate = I("w_gate", [c.NE * c.D + 1, c.DE])
        self.w_up = I("w_up", [c.NE * c.D + 1, c.DE])
        self.w_down = I("w_down", [c.NE * c.DE + 1, c.D])
        self.ln2_g = I("ln2_g", [c.D])
        self.ln2_b = I("ln2_b", [c.D])
        self.y = nc.dram_tensor("y", [c.TPC, c.D], F32, kind="ExternalOutput").ap()
        S = lambda n, s, d=F32: (nc.dram_tensor(n, s, d, kind="ExternalOutput").ap() if n in self.dbg
                                 else nc.dram_tensor(n, s, d).ap())
        self.P = S("P", [c.TPC + 2, c.NIN])
        self.AT = S("AT", [c.TPC, c.AW], BF16)
        self.YF = S("YF", [c.TPC, c.RW])
        self.RWo = S("RWo", [c.TPC, c.RW], BF16)
        self.H = S("H", [c.TPC, c.D])
        self.XB = S("XB", [c.R, c.D], BF16)
        self.YB = S("YB", [c.R, c.D])
        self.DEST = S("DEST", [c.TPC, 4])
        self.IDXS = S("IDXS", [128, c.NB * (c.KT + c.DE // 128)], I32)

    def sb(self, st, name, shape, dt=F32, nb=1):
        return T(st.enter_context(self.nc.sbuf_tensor(name, shape, dt)), nb)

    def ps(self, st, name, shape, dt=F32, nb=1):
        t = T(st.enter_context(self.nc.psum_tensor(name, shape, dt)), nb)
        for b in t.b:
            b.excl = True
        return t

    def phase_proj(self):
        c, nc = self.c, self.nc
        GT = min(16, c.NT)
        with contextlib.ExitStack() as st:
            self.ph = ph = Phase(nc, "A")
            cst = self.sb(st, "A_cst", [128, 128]); identb = self.sb(st, "A_idb", [128, 128], BF16)
            xT = self.sb(st, "A_xT", [128, c.KT, GT * 128], BF16, nb=GT)
            xin = [self.sb(st, f"A_xin{i}", [128, c.D]) for i in range(2)]
            xb = self.sb(st, "A_xb", [128, c.D], BF16)
            CW = 512
            w = [self.sb(st, f"A_w{i}", [128, c.KT, CW], BF16) for i in range(2)]
            stg = [self.sb(st, f"A_stg{i}", [128, CW]) for i in range(2)]
            zero = self.sb(st, "A_zero", [1, c.NIN])
            ptr = [self.ps(st, f"A_ptr{i}", [128, 1024], BF16) for i in range(2)]
            pm = [self.ps(st, f"A_pm{i}", [128, CW]) for i in range(2)]
            self.dma("sp", cst[:, :], self.consts[:, 0, :], (), cst.b)
            self.cp("dve", identb[:, :], cst[:, :], cst.b, identb.b)
            self.memset("pool", zero[:, :], 0.0, zero.b)
            self.dma("sp", self.P[0:1, :], zero[:, :], zero.b, ())
            self.dma("sp", self.P[c.TPC + 1:c.TPC + 2, :], zero[:, :], zero.b, ())
            chunks = [(c0, min(CW, c.NIN - c0)) for c0 in range(0, c.NIN, CW)]
            wi = 0
            ei = 0
            w_view = self.w_in[0:c.D, :].rearrange("(k p) n -> p k n", p=128)
            for g0 in range(0, c.NT, GT):
                gt = min(GT, c.NT - g0)
                for tl in range(gt):
                    j = g0 + tl
                    xi = xin[j % 2]
                    self.dma("sp", xi[:, :], self.x[j * 128:(j + 1) * 128, :], (), xi.b)
                    self.cp("dve", xb[:, :], xi[:, :], xi.b, xb.b)
                    for k0 in range(0, c.KT, 8):
                        kn = min(8, c.KT - k0)
                        pt = ptr[(k0 // 8) % 2]
                        for kk in range(kn):
                            self.tr(pt[:, kk * 128:(kk + 1) * 128], xb[:, (k0 + kk) * 128:(k0 + kk + 1) * 128],
                                    identb[:, :], xb.b + identb.b, pt.b)
                        self.cp("act", xT[:, k0:k0 + kn, tl * 128:(tl + 1) * 128],
                                pt[:, 0:kn * 128].rearrange("p (k t) -> p k t", k=kn), pt.b, [xT.b[tl]])
                for (c0, cw) in chunks:
                    wt = w[wi % 2]
                    wi += 1
                    self.dma("pool", wt[:, :, 0:cw], w_view[:, :, c0:c0 + cw], (), wt.b)
                    for tl in range(gt):
                        j = g0 + tl
                        pp = pm[ei % 2]
                        sg = stg[ei % 2]
                        for k in range(c.KT):
                            self.mm(pp[:, 0:cw], xT[:, k, tl * 128:(tl + 1) * 128], wt[:, k, 0:cw],
                                    k == 0, k == c.KT - 1, [xT.b[tl]] + wt.b, pp.b)
                        self.cp("act" if ei % 2 else "dve", sg[:, 0:cw], pp[:, 0:cw], pp.b, sg.b)
                        self.dma("sp", self.P[1 + j * 128:1 + (j + 1) * 128, c0:c0 + cw], sg[:, 0:cw], sg.b, ())
                        ei += 1
            ph.emit()

    def phase_attn(self):
        c, nc = self.c, self.nc
        NT, AH, KVH, KVW, AW = c.NT, c.AH, c.KVH, c.KVW, c.AW
        with contextlib.ExitStack() as st:
            self.ph = ph = Phase(nc, "B")
            cst = self.sb(st, "B_cst", [128, 128]); identb = self.sb(st, "B_idb", [128, 128], BF16)
            flg = self.sb(st, "B_flg", [128, 6, NT])
            kT = self.sb(st, "B_kT", [64, KVH, (NT + 2) * 128], BF16, nb=NT + 2)
            v = self.sb(st, "B_v", [128, NT + 2, KVW], BF16, nb=NT + 2)
            bias = self.sb(st, "B_bias", [128, AH, 384])
            bim = self.sb(st, "B_bim", [128, 2, 384])
            msk = self.sb(st, "B_msk", [128, 384])
            relb = self.sb(st, "B_relb", [128, 32 * AH])
            sk = self.sb(st, "B_sk", [128, 2, AH])
            kvin = [self.sb(st, f"B_kvin{i}", [128, 2 * KVW]) for i in range(2)]
            kb = self.sb(st, "B_kb", [128, KVW], BF16)
            qin = [self.sb(st, f"B_qin{i}", [128, AW]) for i in range(2)]
            qb = self.sb(st, "B_qb", [128, AW], BF16)
            qT = self.sb(st, "B_qT", [64, AH, 128], BF16)
            L = [self.sb(st, f"B_L{i}", [128, 384]) for i in range(2)]
            pr = [self.sb(st, f"B_p{i}", [128, 384], BF16) for i in range(2)]
            pT = [self.sb(st, f"B_pT{i}", [128, 3, 128], BF16) for i in range(2)]
            stt_ = [self.sb(st, f"B_st{i}", [128, 8]) for i in range(2)]
            ao = [self.sb(st, f"B_ao{i}", [128, AW], BF16) for i in range(2)]
            pq = self.ps(st, "B_pq", [64, 1024], BF16)
            psc = [self.ps(st, f"B_psc{i}", [128, 512]) for i in range(2)]
            ppt = [self.ps(st, f"B_ppt{i}", [128, 8, 128], BF16) for i in range(2)]
            po = [self.ps(st, f"B_po{i}", [128, 512]) for i in range(2)]

            self.dma("sp", cst[:, :], self.consts[:, 0, :], (), cst.b)
            self.cp("dve", identb[:, :], cst[:, :], cst.b, identb.b)
            self.dma("sp", flg[:, :, :], self.flags, (), flg.b)
            self.dma("sp", bim[:, :, :], self.bimat, (), bim.b)
            self.dma("sp", relb[:, :], self.rel_bias.partition_broadcast(128), (), relb.b)
            self.dma("sp", sk[:, 0, :], self.attn_sink.partition_broadcast(128), (), sk.b)
            self.ts("dve", sk[:, 1, :], sk[:, 0, :], -1.0, None, ALU.mult, None, sk.b, sk.b)
            for h in range(AH):
                self.cp("pool", bias[:, h, :], bim[:, 1, :], bim.b, bias.b)
            for b in range(32):
                self.ts("dve", msk[:, :], bim[:, 0, :], float(b), None, ALU.is_equal, None, bim.b, msk.b)
                for h in range(AH):
                    self.stt("dve", bias[:, h, :], msk[:, :], relb[:, b * AH + h:b * AH + h + 1], bias[:, h, :],
                             ALU.mult, ALU.add, msk.b + relb.b + bias.b, bias.b)
            for s in (0, NT + 1):
                self.memset("pool", kT[:, :, s * 128:(s + 1) * 128], 0.0, [kT.b[s]])
                self.memset("pool", v[:, s, :], 0.0, [v.b[s]])
            for j in range(NT):
                kv = kvin[j % 2]
                self.dma("sp", kv[:, :], self.P[1 + j * 128:1 + (j + 1) * 128, AW:AW + 2 * KVW], (), kv.b)
                self.cp("dve", kb[:, :], kv[:, 0:KVW], kv.b, kb.b)
                self.cp("pool", v[:, j + 1, :], kv[:, KVW:2 * KVW], kv.b, [v.b[j + 1]])
                for g in range(KVH):
                    self.tr(pq[:, g * 128:(g + 1) * 128], kb[:, g * 64:(g + 1) * 64], identb[:, :],
                            kb.b + identb.b, pq.b)
                self.cp("act", kT[:, :, (j + 1) * 128:(j + 2) * 128],
                        pq[:, 0:KVH * 128].rearrange("p (g t) -> p g t", g=KVH), pq.b, [kT.b[j + 1]])
            it = 0
            for j in range(NT):
                qi = qin[j % 2]
                a_o = ao[j % 2]
                self.dma("sp", qi[:, :], self.P[1 + j * 128:1 + (j + 1) * 128, 0:AW], (), qi.b)
                self.ts("dve", qb[:, :], qi[:, :], 0.125, None, ALU.mult, None, qi.b, qb.b)
                for h0 in range(0, AH, 8):
                    hn = min(8, AH - h0)
                    for hh in range(hn):
                        h = h0 + hh
                        self.tr(pq[:, hh * 128:(hh + 1) * 128], qb[:, h * 64:(h + 1) * 64], identb[:, :],
                                qb.b + identb.b, pq.b)
                    self.cp("act", qT[:, h0:h0 + hn, :], pq[:, 0:hn * 128].rearrange("p (g t) -> p g t", g=hn),
                            pq.b, qT.b)
                for h in range(AH):
                    g = h // 4
                    sc, Lt, pt, pTt, sx, ppt_, po_ = (psc[it % 2], L[it % 2], pr[it % 2], pT[it % 2],
                                                      stt_[it % 2], ppt[it % 2], po[it % 2])
                    it += 1
                    self.mm(sc[:, 0:384], qT[:, h, :], kT[:, g, j * 128:(j + 3) * 128], True, True,
                            qT.b + [kT.b[j], kT.b[j + 1], kT.b[j + 2]], sc.b)
                    self.tt("dve", Lt[:, :], sc[:, 0:384], bias[:, h, :], ALU.add, sc.b + bias.b, Lt.b)
                    self.ts("dve", Lt[:, 0:128], Lt[:, 0:128], flg[:, 2, j:j + 1], None, ALU.add, None,
                            Lt.b + flg.b, Lt.b)
                    self.ts("dve", Lt[:, 256:384], Lt[:, 256:384], flg[:, 3, j:j + 1], None, ALU.add, None,
                            Lt.b + flg.b, Lt.b)
                    self.red(sx[:, 0:1], Lt[:, :], ALU.max, Lt.b, sx.b, negate=True)
                    self.tt("dve", sx[:, 1:2], sx[:, 0:1], sk[:, 1, h:h + 1], ALU.min, sx.b + sk.b, sx.b)
                    self.act(pt[:, :], Lt[:, :], AF.Exp, Lt.b + sx.b, pt.b + sx.b, bias=sx[:, 1:2],
                             accum=sx[:, 2:3])
                    self.act(sx[:, 3:4], sk[:, 0, h:h + 1], AF.Exp, sk.b + sx.b, sx.b, bias=sx[:, 1:2])
                    self.tt("dve", sx[:, 4:5], sx[:, 2:3], sx[:, 3:4], ALU.add, sx.b, sx.b)
                    self.ph.op("dve", lambda e, o=sx[:, 5:6], i=sx[:, 4:5]: e.reciprocal(out=o, in_=i), sx.b, sx.b)
                    for kbk in range(3):
                        self.tr(ppt_[:, kbk, :], pt[:, kbk * 128:(kbk + 1) * 128], identb[:, :],
                                pt.b + identb.b, ppt_.b)
                    self.cp("act", pTt[:, :, :], ppt_[:, 0:3, :], ppt_.b, pTt.b)
                    for kbk in range(3):
                        self.mm(po_[:, 0:64], pTt[:, kbk, :], v[:, j + kbk, g * 64:(g + 1) * 64], kbk == 0, kbk == 2,
                                pTt.b + [v.b[j + kbk]], po_.b)
                    self.ts("dve", a_o[:, h * 64:(h + 1) * 64], po_[:, 0:64], sx[:, 5:6], None, ALU.mult, None,
                            po_.b + sx.b, a_o.b)
                self.dma("sp", self.AT[j * 128:(j + 1) * 128, :], a_o[:, :], a_o.b, ())
            ph.emit()

    def phase_rwkv(self, d):
        c, nc = self.c, self.nc
        RW, RH, NT, GL, DL, IL = c.RW, c.RH, c.NT, c.GL, c.DL, c.IL
        final = (d == 1)
        mS, mI, mQ = (1, 2, 3) if d == 0 else (3, 4, 1)
        LW = GL + 2 * DL + 2 * IL
        SW = max(RW, LW)
        PC = c.AW + 2 * c.KVW
        NBH = min(4, RH)
        HP = RH // 2
        with contextlib.ExitStack() as st:
            self.ph = ph = Phase(nc, "C%d" % d)
            sb = lambda n, s, dt=F32, nb=1: self.sb(st, "C%d_" % d + n, s, dt, nb)
            cst = sb("cst", [128, 8, 128]); identb = sb("idb", [128, 128], BF16)
            flg = sb("flg", [128, 6, NT])
            tiny = sb("tiny", [128, 1])
            self.memset("pool", tiny[:, :], 1e-24, tiny.b)
            mu = sb("mu", [128, c.RC])
            w0b = sb("w0b", [128, RW]); a0b = sb("a0b", [128, RW])
            kkp = sb("kkp", [128, RW]); kap = sb("kap", [128, RW]); oka = sb("oka", [128, RW])
            w2d = sb("w2d", [64, RW], BF16); a2d = sb("a2d", [64, RW], BF16)
            if final:
                a0f = sb("a0f", [128, RW]); a2f = sb("a2f", [64, RW], BF16)
                g2 = sb("g2", [128, 2, RW], BF16)
                rkb = sb("rkb", [128, RW]); gng = sb("gng", [128, RW]); gnb = sb("gnb", [128, RW])
                yf = sb("yf", [128, RW])
                sm = sb("sm", [128, 8, RH]); zg = sb("zg", [128, GL], BF16)
                rwo = sb("rwo", [128, RW], BF16)
            cur = sb("cur", [128, SW]); prv = sb("prv", [128, SW]); nxt = sb("nxt", [128, SW])
            tmp = sb("tmp", [128, SW])
            sr = sb("sr", [128, RW]); skk = sb("skk", [128, RW]); sv = sb("sv", [128, RW]); sl = sb("sl", [128, LW])
            zz = sb("zz", [128, 3, 64], BF16); zT = sb("zT", [128, 5, 128], BF16)
            f1 = sb("f1", [128, RW]); f2 = sb("f2", [128, RW]); f3 = sb("f3", [128, RW]); f4 = sb("f4", [128, RW])
            lw = sb("lw", [128, RW]); av = sb("av", [128, RW]); kd = sb("kd", [128, RW]); kkn = sb("kkn", [128, RW])
            bv = sb("bv", [128, RW]); ssq = sb("ssq", [128, 2, RH])
            E1 = sb("E1", [128, RW]); E2 = sb("E2", [128, RW]); E3 = f1; E4 = f4; E5 = f3
            af = E1; kdf = E2
            ra = sb("ra", [128, RW], BF16); ka = sb("ka", [128, RW], BF16); ba = sb("ba", [128, RW], BF16)
            aa = sb("aa", [128, RW], BF16); kg = sb("kg", [128, RW], BF16); bg = sb("bg", [128, RW], BF16)
            Vb = sb("Vb", [128, RW], BF16); E5d = sb("E5d", [128, RW], BF16)
            FT = sb("FT", [64, RH, 4, 128], BF16)
            Yacc = sb("Yacc", [128, RW])
            St = [sb(f"St{i}", [64, RH, 64], BF16) for i in range(2)]
            Aab = sb("Aab", [128, NBH, 128], BF16); Arb = sb("Arb", [128, NBH, 128], BF16)
            Aak = sb("Aak", [128, NBH, 128], BF16); Ark = sb("Ark", [128, NBH, 128], BF16)
            Pm = [sb(f"Pm{i}", [128, NBH, 128], BF16) for i in range(2)]
            Qm = [sb(f"Qm{i}", [128, NBH, 128], BF16) for i in range(2)]
            Qp = sb("Qp", [128, NBH, 128], BF16)
            Inv = [sb(f"Inv{i}", [128, NBH, 128], BF16) for i in range(2)]
            W1 = sb("W1", [128, NBH, 64], BF16); AU = sb("AU", [128, NBH, 128], BF16)
            RpT = sb("RpT", [64, NBH, 128], BF16); MT = sb("MT", [64, NBH, 64], BF16)
            pb0 = self.ps(st, "C%d_pb0" % d, [128, 1024]); pb1 = self.ps(st, "C%d_pb1" % d, [128, 1024], nb=2)
            pI = self.ps(st, "C%d_pI" % d, [128, 512]); pW = self.ps(st, "C%d_pW" % d, [128, 512])
            pR = self.ps(st, "C%d_pR" % d, [128, 512]); pG = self.ps(st, "C%d_pG" % d, [128, 512])
            pWb = pW[:, :].bitcast(BF16); pIb = pI[:, :].bitcast(BF16)

            self.dma("sp", cst[:, :, :], self.consts, (), cst.b)
            self.cp("dve", identb[:, :], cst[:, 0, :], cst.b, identb.b)
            self.dma("sp", flg[:, :, :], self.flags, (), flg.b)
            bc = lambda ap: ap.partition_broadcast(128)
            self.dma("sp", mu[:, :], bc(self.shift_mu), (), mu.b)
            self.dma("sp", w0b[:, :], bc(self.decay_w0[d]), (), w0b.b)
            self.dma("sp", a0b[:, :], bc(self.iclr_a0[d]), (), a0b.b)
            self.dma("sp", kkp[:, :], bc(self.k_k), (), kkp.b)
            self.dma("sp", kap[:, :], bc(self.k_a), (), kap.b)
            self.ts("dve", oka[:, :], kap[:, :], -1.0, 1.0, ALU.mult, ALU.add, kap.b, oka.b)
            if self.cut == 11:
                ph.emit()
                return
            self.dma("pool", w2d[:, :], self.decay_w2[d], (), w2d.b)
            self.dma("pool", a2d[:, :], self.iclr_a2[d], (), a2d.b)
            if final:
                self.dma("sp", a0f[:, :], bc(self.iclr_a0[0]), (), a0f.b)
                self.dma("pool", a2f[:, :], self.iclr_a2[0], (), a2f.b)
                self.dma("pool", g2[:, 0, :], self.gate_g2[0:128, :], (), g2.b)
                self.dma("pool", g2[0:GL - 128, 1, :], self.gate_g2[128:GL, :], (), g2.b)
                self.dma("sp", rkb[:, :], bc(self.r_k), (), rkb.b)
                self.dma("sp", gng[:, :], bc(self.gn_g), (), gng.b)
                self.dma("sp", gnb[:, :], bc(self.gn_b), (), gnb.b)
            for s in St:
                self.memset("pool", s[:, :, :], 0.0, s.b)
            if self.cut == 10:
                ph.emit()
                return

            def shift(j, c0, wd, dest):
                r0 = 1 + j * 128
                col = PC + c0
                self.dma("sp", cur[:, 0:wd], self.P[r0:r0 + 128, col:col + wd], (), cur.b)
                self.dma("sp", prv[:, 0:wd], self.P[r0 - 1:r0 + 127, col:col + wd], (), prv.b)
                self.dma("sp", nxt[:, 0:wd], self.P[r0 + 1:r0 + 129, col:col + wd], (), nxt.b)
                self.ts("pool", tmp[:, 0:wd], prv[:, 0:wd], flg[:, 0, j:j + 1], None, ALU.mult, None,
                        prv.b + flg.b, tmp.b)
                self.stt("dve", tmp[:, 0:wd], nxt[:, 0:wd], flg[:, 1, j:j + 1], tmp[:, 0:wd], ALU.mult, ALU.add,
                         nxt.b + flg.b + tmp.b, tmp.b)
                self.stt("dve", tmp[:, 0:wd], tmp[:, 0:wd], 0.5, cur[:, 0:wd], ALU.mult, ALU.subtract,
                         tmp.b + cur.b, tmp.b)
                self.tt("pool", tmp[:, 0:wd], tmp[:, 0:wd], mu[:, c0:c0 + wd], ALU.mult, tmp.b + mu.b, tmp.b)
                self.tt("dve", dest[:, 0:wd], tmp[:, 0:wd], cur[:, 0:wd], ALU.add, tmp.b + cur.b, dest.b)

            hv = lambda t: t[:, :].rearrange("p (h k) -> p h k", k=64)
            order = list(range(NT)) if d == 0 else list(range(NT - 1, -1, -1))
            for oi, j in enumerate(order):
                So, Sn = St[oi % 2], St[(oi + 1) % 2]
                jn = order[oi + 1] if oi + 1 < NT else None
                shift(j, 0, RW, sr); shift(j, RW, RW, skk); shift(j, 2 * RW, RW, sv); shift(j, 3 * RW, LW, sl)
                if self.cut == 1:
                    ph.emit()
                    return
                self.act(zz[:, 0, :], sl[:, GL + d * DL:GL + (d + 1) * DL], AF.Tanh, sl.b, zz.b)
                self.cp("dve", zz[:, 1, :], sl[:, GL + 2 * DL + d * IL:GL + 2 * DL + (d + 1) * IL], sl.b, zz.b)
                self.tr(pWb[0:64, 0:128], zz[:, 0, :], identb[:, :], zz.b + identb.b, pW.b)
                self.tr(pWb[0:64, 128:256], zz[:, 1, :], identb[:, :], zz.b + identb.b, pW.b)
                nz = 2
                if final:
                    self.cp("dve", zz[:, 2, :], sl[:, GL + 2 * DL:GL + 2 * DL + IL], sl.b, zz.b)
                    self.act(zg[:, :], sl[:, 0:GL], AF.Sigmoid, sl.b, zg.b)
                    self.tr(pWb[0:64, 256:384], zz[:, 2, :], identb[:, :], zz.b + identb.b, pW.b)
                    self.tr(pWb[:, 384:512], zg[:, 0:128], identb[:, :], zg.b + identb.b, pW.b)
                    self.tr(pWb[0:GL - 128, 512:640], zg[:, 128:GL], identb[:, :], zg.b + identb.b, pW.b)
                    nz = 5
                n64 = 3 if final else 2
                self.cp("act", zT[0:64, 0:n64, :], pWb[0:64, 0:n64 * 128].rearrange("p (a t) -> p a t", a=n64),
                        pW.b, zT.b)
                if final:
                    self.cp("act", zT[:, 3, :], pWb[:, 384:512], pW.b, zT.b)
                    self.cp("act", zT[0:GL - 128, 4, :], pWb[0:GL - 128, 512:640], pW.b, zT.b)
                for n0 in range(0, RW, 512):
                    nn = min(512, RW - n0)
                    self.mm(pb0[:, n0:n0 + nn], zT[0:64, 0, :], w2d[:, n0:n0 + nn], True, True, zT.b + w2d.b, pb0.b)
                    self.mm(pb1[:, n0:n0 + nn], zT[0:64, 1, :], a2d[:, n0:n0 + nn], True, True, zT.b + a2d.b, pb1.b)
                self.tt("dve", f1[:, :], pb0[:, 0:RW], w0b[:, :], ALU.add, pb0.b + w0b.b, f1.b)
                self.act(lw[:, :], f1[:, :], AF.Sigmoid, f1.b, lw.b)
                self.ts("pool", lw[:, :], lw[:, :], -math.exp(-0.5), None, ALU.mult, None, lw.b, lw.b)
                self.tt("dve", f2[:, :], pb1[:, 0:RW], a0b[:, :], ALU.add, pb1.b + a0b.b, f2.b)
                self.act(av[:, :], f2[:, :], AF.Sigmoid, f2.b, av.b)
                self.tt("pool", f1[:, :], av[:, :], kap[:, :], ALU.mult, av.b + kap.b, f1.b)
                self.tt("pool", f1[:, :], f1[:, :], oka[:, :], ALU.add, f1.b + oka.b, f1.b)
                self.tt("dve", kd[:, :], skk[:, :], f1[:, :], ALU.mult, skk.b + f1.b, kd.b)
                self.tt("pool", f2[:, :], skk[:, :], kkp[:, :], ALU.mult, skk.b + kkp.b, f2.b)
                self.tt("pool", f3[:, :], f2[:, :], f2[:, :], ALU.mult, f2.b, f3.b)
                self.red(ssq[:, 0, :], hv(f3), ALU.add, f3.b, ssq.b)
                self.act(ssq[:, 1, :], ssq[:, 0, :], AF.Sqrt, ssq.b, ssq.b, bias=tiny[:, 0:1])
                self.ph.op("dve", lambda e, o=ssq[:, 1, :], i=ssq[:, 1, :]: e.reciprocal(out=o, in_=i), ssq.b, ssq.b)
                self.tt("dve", hv(kkn), hv(f2), ssq[:, 1, :].unsqueeze(2).to_broadcast([128, RH, 64]), ALU.mult,
                        f2.b + ssq.b, kkn.b)
                self.tt("pool", bv[:, :], kkn[:, :], av[:, :], ALU.mult, kkn.b + av.b, bv.b)
                if self.cut == 2:
                    ph.emit()
                    return
                for n0 in range(0, RW, 512):
                    nn = min(512, RW - n0)
                    self.mm(pb0[:, n0:n0 + nn], cst[:, mI, :], lw[:, n0:n0 + nn], True, True, cst.b + lw.b, pb0.b)
                    self.mm(pb1[:, n0:n0 + nn], cst[:, 5, :], lw[:, n0:n0 + nn], True, True, cst.b + lw.b, pb1.b)
                self.act(E1[:, :], pb0[:, 0:RW], AF.Exp, pb0.b, E1.b)
                self.act(E2[:, :], pb0[:, 0:RW], AF.Exp, pb0.b, E2.b, scale=-1.0)
                if self.cut == 31:
                    ph.emit()
                    return
                self.tt("dve", f1[:, :], pb0[:, 0:RW], lw[:, :], ALU.subtract, pb0.b + lw.b, f1.b)
                if self.cut == 321:
                    ph.emit()
                    return
                self.cp("act", f3[:, :], pb1[:, 0:RW], pb1.b, f3.b)
                if self.cut == 322:
                    ph.emit()
                    return
                self.tt("pool", f4[:, :], f3[:, :], f1[:, :], ALU.subtract, f3.b + f1.b, f4.b)
                self.tt("pool", f4[:, :], f4[:, :], lw[:, :], ALU.subtract, f4.b + lw.b, f4.b)
                if self.cut == 323:
                    ph.emit()
                    return
                self.act(E3[:, :], f1[:, :], AF.Exp, f1.b, E3.b)
                if self.cut == 324:
                    ph.emit()
                    return
                self.act(E4[:, :], f4[:, :], AF.Exp, f4.b, E4.b)
                self.act(E5[:, :], f3[:, :], AF.Exp, f3.b, E5.b)
                if self.cut == 32:
                    ph.emit()
                    return
                self.tt("dve", ra[:, :], sr[:, :], E1[:, :], ALU.mult, sr.b + E1.b, ra.b)
                self.tt("pool", ka[:, :], kd[:, :], E2[:, :], ALU.mult, kd.b + E2.b, ka.b)
                self.tt("dve", ba[:, :], bv[:, :], E2[:, :], ALU.mult, bv.b + E2.b, ba.b)
                self.stt("dve", aa[:, :], kkn[:, :], -1.0, E3[:, :], ALU.mult, ALU.mult, kkn.b + E3.b, aa.b)
                self.tt("pool", kg[:, :], kd[:, :], E4[:, :], ALU.mult, kd.b + E4.b, kg.b)
                self.tt("dve", bg[:, :], bv[:, :], E4[:, :], ALU.mult, bv.b + E4.b, bg.b)
                if self.cut == 33:
                    ph.emit()
                    return
                self.cp("pool", Vb[:, :], sv[:, :], sv.b, Vb.b)
                self.tt("pool", hv(E5d), hv(E5), cst[:, 0, 0:64].unsqueeze(1).to_broadcast([128, RH, 64]), ALU.mult,
                        E5.b + cst.b, E5d.b)
                if self.cut == 3:
                    ph.emit()
                    return
                srcs = (aa, ra, ba, ka)
                for hp in range(HP):
                    pp, ppb = (pI, pIb) if hp % 2 == 0 else (pW, pWb)
                    for h2 in range(2):
                        for q in range(4):
                            hq = hp * 2 + h2
                            self.tr(ppb[0:64, (h2 * 4 + q) * 128:(h2 * 4 + q + 1) * 128],
                                    srcs[q][:, hq * 64:(hq + 1) * 64], identb[:, :], srcs[q].b + identb.b, pp.b)
                    self.cp("act" if hp % 2 else "dve", FT[:, hp * 2:hp * 2 + 2, :, :],
                            ppb[0:64, 0:1024].rearrange("p (h q t) -> p h q t", h=2, q=4), pp.b, FT.b)
                    if self.cut == 4:
                        ph.emit()
                        return
                for h0 in range(0, RH, NBH):
                    hs = list(range(h0, h0 + NBH))
                    fb = lambda h: 0
                    bcm = lambda m: cst[:, m, :].unsqueeze(1).to_broadcast([128, NBH, 128])
                    pA4 = pb0[:, :].rearrange("p (a b) -> p a b", a=4)
                    v4 = lambda t: t[:, :].rearrange("p (a b) -> p a b", a=4)
                    for (q, dA, dR) in ((2, Aab, Arb), (3, Aak, Ark)):
                        for hh, h in enumerate(hs):
                            b0 = fb(h)
                            self.mm(pA4[:, hh, :], FT[:, h, q, :],
                                    FT[:, h, 0:2, :].rearrange("p a t -> p (a t)"),
                                    True, True, FT.b, pb0.b)
                        self.tt("dve", dA[:, :, :], pA4[:, 0:NBH, 0:128], bcm(mS), ALU.mult, pb0.b + cst.b, dA.b)
                        self.tt("dve", dR[:, :, :], pA4[:, 0:NBH, 128:256], bcm(mI), ALU.mult, pb0.b + cst.b, dR.b)
                        if self.cut == 5:
                            ph.emit()
                            return
                    pP4 = pb1[:, 0:512].rearrange("p (a b) -> p a b", a=4)
                    pQ4 = pb1[:, 512:1024].rearrange("p (a b) -> p a b", a=4)
                    pI4 = v4(pI)
                    for hh, h in enumerate(hs):
                        b0 = fb(h)
                        self.mm(pQ4[:, hh, :], FT[:, h, 0, :], FT[:, h, 2, :],
                                True, True, FT.b, [pb1.b[1]])
                    Qc = Qm[0]
                    self.tt("dve", Qc[:, :, :], pQ4[:, 0:NBH, :], bcm(mQ), ALU.mult, [pb1.b[1]] + cst.b, Qc.b)
                    idb = identb[:, :].unsqueeze(1).to_broadcast([128, NBH, 128])
                    Ic = Inv[0]
                    self.tt("pool", Ic[:, :, :], Aab[:, :, :], idb, ALU.add, Aab.b + identb.b, Ic.b)
                    Pc = Aab
                    for it in range(1, 7):
                        Pn, Qn, In = Pm[it % 2], Qm[it % 2], Inv[it % 2]
                        if it <= 5:
                            for hh in range(NBH):
                                self.mm(pP4[:, hh, :], Qc[:, hh, :], Pc[:, hh, :], True, True, Qc.b + Pc.b, [pb1.b[0]])
                        for hh in range(NBH):
                            self.mm(pQ4[:, hh, :], Pc[:, hh, :], Qc[:, hh, :], True, True, Qc.b + Pc.b, [pb1.b[1]])
                        if it <= 5:
                            self.cp("act", Pn[:, :, :], pP4[:, 0:NBH, :], [pb1.b[0]], Pn.b)
                        self.cp("dve", Qn[:, :, :], pQ4[:, 0:NBH, :], [pb1.b[1]], Qn.b)
                        self.tt("pool", Qp[:, :, :], Qn[:, :, :], idb, ALU.add, Qn.b + identb.b, Qp.b)
                        for hh in range(NBH):
                            self.mm(pI4[:, hh, :], Qp[:, hh, :], Ic[:, hh, :], True, True, Qp.b + Ic.b, pI.b)
                        self.cp("act", In[:, :, :], pI4[:, 0:NBH, :], pI.b, In.b)
                        Pc, Qc, Ic = Pn, Qn, In
                        if self.cut == 6:
                            ph.emit()
                            return
                    pW4 = v4(pW)
                    for hh, h in enumerate(hs):
                        self.mm(pW4[:, hh, 0:64], Aak[:, hh, :], Vb[:, h * 64:(h + 1) * 64], True, True,
                                Aak.b + Vb.b, pW.b)
                    self.cp("act", W1[:, :, :], pW4[:, 0:NBH, 0:64], pW.b, W1.b)
                    for hh, h in enumerate(hs):
                        self.mm(pW4[:, hh, 0:64], Ic[:, hh, :], aa[:, h * 64:(h + 1) * 64], True, True,
                                Ic.b + aa.b, pW.b)
                        self.mm(pW4[:, hh, 64:128], Ic[:, hh, :], W1[:, hh, :], True, True, Ic.b + W1.b, pW.b)
                    self.cp("dve", AU[:, :, :], pW4[:, 0:NBH, :], pW.b, AU.b)
                    pR4 = pR[0:64, :].rearrange("p (a b) -> p a b", a=4)
                    for hh, h in enumerate(hs):
                        self.mm(pR4[:, hh, :], AU[:, hh, 0:64], Arb[:, hh, :], True, False, AU.b + Arb.b, pR.b)
                        self.mm(pR4[:, hh, :], ra[:, h * 64:(h + 1) * 64], identb[:, :], False, True,
                                ra.b + identb.b, pR.b)
                    self.cp("act", RpT[:, :, :], pR4[:, 0:NBH, :], pR.b, RpT.b)
                    pY4 = pG[:, 0:256].rearrange("p (a b) -> p a b", a=4)
                    for hh, h in enumerate(hs):
                        self.mm(pY4[:, hh, :], Arb[:, hh, :], AU[:, hh, 64:128], True, False, Arb.b + AU.b, pG.b)
                        self.mm(pY4[:, hh, :], Ark[:, hh, :], Vb[:, h * 64:(h + 1) * 64], False, False,
                                Ark.b + Vb.b, pG.b)
                        self.mm(pY4[:, hh, :], RpT[:, hh, :], So[:, h, :], False, True, RpT.b + So.b, pG.b)
                    self.cp("dve", Yacc[:, h0 * 64:(h0 + NBH) * 64].rearrange("p (a b) -> p a b", a=NBH),
                            pY4[:, 0:NBH, :], pG.b, Yacc.b)
                    pM4 = pR[0:64, 0:256].rearrange("p (a b) -> p a b", a=4)
                    for hh, h in enumerate(hs):
                        self.mm(pM4[:, hh, :], AU[:, hh, 0:64], bg[:, h * 64:(h + 1) * 64], True, False,
                                AU.b + bg.b, pR.b)
                        self.mm(pM4[:, hh, :], E5d[:, h * 64:(h + 1) * 64], identb[:, 0:64], False, True,
                                E5d.b + identb.b, pR.b)
                    self.cp("act", MT[:, :, :], pM4[:, 0:NBH, :], pR.b, MT.b)
                    pS4 = pI[0:64, 0:256].rearrange("p (a b) -> p a b", a=4)
                    for hh, h in enumerate(hs):
                        self.mm(pS4[:, hh, :], bg[:, h * 64:(h + 1) * 64], AU[:, hh, 64:128], True, False,
                                bg.b + AU.b, pI.b)
                        self.mm(pS4[:, hh, :], kg[:, h * 64:(h + 1) * 64], Vb[:, h * 64:(h + 1) * 64], False, False,
                                kg.b + Vb.b, pI.b)
                        self.mm(pS4[:, hh, :], MT[:, hh, :], So[:, h, :], False, True, MT.b + So.b, pI.b)
                    if jn is not None:
                        self.ts("dve", Sn[:, h0:h0 + NBH, :], pS4[:, 0:NBH, :], flg[0:64, 4 + d, jn:jn + 1], None,
                                ALU.mult, None, pI.b + flg.b, Sn.b)
                        if self.cut == 7:
                            ph.emit()
                            return
                if not final:
                    self.dma("sp", self.YF[j * 128:(j + 1) * 128, :], Yacc[:, :], Yacc.b, ())
                    continue
                self.dma("sp", yf[:, :], self.YF[j * 128:(j + 1) * 128, :], (), yf.b)
                for n0 in range(0, RW, 512):
                    nn = min(512, RW - n0)
                    self.mm(pb0[:, n0:n0 + nn], zT[0:64, 2, :], a2f[:, n0:n0 + nn], True, True, zT.b + a2f.b, pb0.b)
                self.tt("dve", f1[:, :], pb0[:, 0:RW], a0f[:, :], ALU.add, pb0.b + a0f.b, f1.b)
                self.act(af[:, :], f1[:, :], AF.Sigmoid, f1.b, af.b)
                self.tt("pool", f1[:, :], af[:, :], kap[:, :], ALU.mult, af.b + kap.b, f1.b)
                self.tt("pool", f1[:, :], f1[:, :], oka[:, :], ALU.add, f1.b + oka.b, f1.b)
                self.tt("dve", kdf[:, :], skk[:, :], f1[:, :], ALU.mult, skk.b + f1.b, kdf.b)
                self.tt("pool", f2[:, :], sr[:, :], rkb[:, :], ALU.mult, sr.b + rkb.b, f2.b)
                self.tt("pool", f3[:, :], kdf[:, :], kd[:, :], ALU.add, kdf.b + kd.b, f3.b)
                self.tt("dve", f3[:, :], f3[:, :], f2[:, :], ALU.mult, f3.b + f2.b, f3.b)
                self.red(sm[:, 0, :], hv(f3), ALU.add, f3.b, sm.b)
                bch = lambda k: sm[:, k, :].unsqueeze(2).to_broadcast([128, RH, 64])
                self.tt("dve", hv(f4), hv(sv), bch(0), ALU.mult, sv.b + sm.b, f4.b)
                self.tt("pool", f4[:, :], f4[:, :], yf[:, :], ALU.add, f4.b + yf.b, f4.b)
                self.tt("dve", f4[:, :], f4[:, :], Yacc[:, :], ALU.add, f4.b + Yacc.b, f4.b)
                self.red(sm[:, 1, :], hv(f4), ALU.add, f4.b, sm.b)
                self.ts("dve", sm[:, 2, :], sm[:, 1, :], 1.0 / 64, None, ALU.mult, None, sm.b, sm.b)
                self.tt("dve", hv(f1), hv(f4), bch(2), ALU.subtract, f4.b + sm.b, f1.b)
                self.tt("pool", f2[:, :], f1[:, :], f1[:, :], ALU.mult, f1.b, f2.b)
                self.red(sm[:, 3, :], hv(f2), ALU.add, f2.b, sm.b)
                self.ts("dve", sm[:, 4, :], sm[:, 3, :], 1.0 / 64, 64e-5, ALU.mult, ALU.add, sm.b, sm.b)
                self.act(sm[:, 5, :], sm[:, 4, :], AF.Sqrt, sm.b, sm.b)
                self.ph.op("dve", lambda e, o=sm[:, 5, :], i=sm[:, 5, :]: e.reciprocal(out=o, in_=i), sm.b, sm.b)
                self.tt("dve", hv(f3), hv(f1), bch(5), ALU.mult, f1.b + sm.b, f3.b)
                self.tt("pool", f3[:, :], f3[:, :], gng[:, :], ALU.mult, f3.b + gng.b, f3.b)
                self.tt("pool", f3[:, :], f3[:, :], gnb[:, :], ALU.add, f3.b + gnb.b, f3.b)
                for n0 in range(0, RW, 512):
                    nn = min(512, RW - n0)
                    self.mm(pb1[:, n0:n0 + nn], zT[:, 3, :], g2[:, 0, n0:n0 + nn], True, False, zT.b + g2.b, pb1.b)
                    self.mm(pb1[:, n0:n0 + nn], zT[0:GL - 128, 4, :], g2[0:GL - 128, 1, n0:n0 + nn], False, True,
                            zT.b + g2.b, pb1.b)
                self.tt("dve", rwo[:, :], pb1[:, 0:RW], f3[:, :], ALU.mult, f3.b + pb1.b, rwo.b)
                self.dma("sp", self.RWo[j * 128:(j + 1) * 128, :], rwo[:, :], rwo.b, ())
            ph.emit()

    def layer_norm(self, src, dst, gb, bb, sm, scr, D):
        self.red(sm[:, 0:1], src[:, :], ALU.add, src.b, sm.b)
        self.ts("dve", sm[:, 1:2], sm[:, 0:1], -1.0 / D, None, ALU.mult, None, sm.b, sm.b)
        self.ts("dve", dst[:, :], src[:, :], sm[:, 1:2], None, ALU.add, None, src.b + sm.b, dst.b)
        self.act(scr[:, :], dst[:, :], AF.Square, dst.b + sm.b, scr.b + sm.b, accum=sm[:, 2:3])
        self.ts("dve", sm[:, 3:4], sm[:, 2:3], 1.0 / D, 1e-5, ALU.mult, ALU.add, sm.b, sm.b)
        self.act(sm[:, 4:5], sm[:, 3:4], AF.Sqrt, sm.b, sm.b)
        self.ph.op("dve", lambda e, o=sm[:, 4:5], i=sm[:, 4:5]: e.reciprocal(out=o, in_=i), sm.b, sm.b)
        self.ts("dve", dst[:, :], dst[:, :], sm[:, 4:5], None, ALU.mult, None, dst.b + sm.b, dst.b)
        self.tt("pool", dst[:, :], dst[:, :], gb[:, :], ALU.mult, dst.b + gb.b, dst.b)
        self.tt("dve", dst[:, :], dst[:, :], bb[:, :], ALU.add, dst.b + bb.b, dst.b)

    def phase_out(self):
        c, nc = self.c, self.nc
        D, KT, NT = c.D, c.KT, c.NT
        with contextlib.ExitStack() as st:
            self.ph = ph = Phase(nc, "D")
            sb = lambda n, s, dt=F32, nb=1: self.sb(st, "D_" + n, s, dt, nb)
            cst = sb("cst", [128, 128]); identb = sb("idb", [128, 128], BF16)
            wo = sb("wo", [128, KT, D], BF16)
            gb = sb("gb", [128, D]); bb = sb("bb", [128, D])
            mi = [sb(f"mi{i}", [128, D], BF16) for i in range(2)]
            mT = sb("mT", [128, KT, 128], BF16)
            xi = [sb(f"xi{i}", [128, D]) for i in range(2)]
            res = sb("res", [128, D]); hh = [sb(f"hh{i}", [128, D]) for i in range(2)]
            scr = sb("scr", [128, D]); sm = sb("sm", [128, 8])
            ptr = [self.ps(st, f"D_ptr{i}", [128, 1024], BF16) for i in range(2)]
            pm = self.ps(st, "D_pm", [128, D])
            self.dma("sp", cst[:, :], self.consts[:, 0, :], (), cst.b)
            self.cp("dve", identb[:, :], cst[:, :], cst.b, identb.b)
            wv = self.w_out[0:D, :].rearrange("(k p) n -> p k n", p=128)
            for k in range(KT):
                self.dma("pool", wo[:, k, :], wv[:, k, :], (), wo.b)
            self.dma("sp", gb[:, :], self.ln1_g.partition_broadcast(128), (), gb.b)
            self.dma("sp", bb[:, :], self.ln1_b.partition_broadcast(128), (), bb.b)
            for j in range(NT):
                m, x, h = mi[j % 2], xi[j % 2], hh[j % 2]
                self.dma("sp", m[:, 0:c.AW], self.AT[j * 128:(j + 1) * 128, :], (), m.b)
                self.dma("sp", m[:, c.AW:D], self.RWo[j * 128:(j + 1) * 128, :], (), m.b)
                self.dma("sp", x[:, :], self.x[j * 128:(j + 1) * 128, :], (), x.b)
                for k0 in range(0, KT, 8):
                    kn = min(8, KT - k0)
                    pt = ptr[(k0 // 8) % 2]
                    for kk in range(kn):
                        self.tr(pt[:, kk * 128:(kk + 1) * 128], m[:, (k0 + kk) * 128:(k0 + kk + 1) * 128],
                                identb[:, :], m.b + identb.b, pt.b)
                    self.cp("act", mT[:, k0:k0 + kn, :], pt[:, 0:kn * 128].rearrange("p (k t) -> p k t", k=kn),
                            pt.b, mT.b)
                for n0 in range(0, D, 512):
                    nn = min(512, D - n0)
                    for k in range(KT):
                        self.mm(pm[:, n0:n0 + nn], mT[:, k, :], wo[:, k, n0:n0 + nn], k == 0, k == KT - 1,
                                mT.b + wo.b, pm.b)
                self.ts("pool", x[:, :], x[:, :], c.alpha, None, ALU.mult, None, x.b, x.b)
                self.tt("dve", res[:, :], pm[:, :], x[:, :], ALU.add, x.b + pm.b, res.b)
                self.layer_norm(res, h, gb, bb, sm, scr, D)
                self.dma("sp", self.H[j * 128:(j + 1) * 128, :], h[:, :], h.b, ())
            ph.emit()

    def phase_route(self):
        c, nc = self.c, self.nc
        D, KT, NT, NG, EPG, NE, NB = c.D, c.KT, c.NT, c.NG, c.EPG, c.NE, c.NB
        FT = c.DE // 128
        NL = NG + NE
        with contextlib.ExitStack() as st:
            self.ph = ph = Phase(nc, "E")
            sb = lambda n, s, dt=F32, nb=1: self.sb(st, "E_" + n, s, dt, nb)
            cst = sb("cst", [128, 8, 128]); cstb = sb("cstb", [128, 2, 128], BF16)
            c2 = sb("c2", [128, NB + KT + FT])
            wr = sb("wr", [128, KT, NL]); brb = sb("brb", [128, NL])
            hi = [sb(f"hi{i}", [128, D]) for i in range(2)]
            hT = sb("hT", [128, KT, 128])
            lg = sb("lg", [128, NL]); sm = sb("sm", [128, 16])
            ohg = sb("ohg", [128, NG]); t3 = sb("t3", [128, NG, EPG]); es = sb("es", [128, 2, EPG])
            oh = sb("oh", [128, 2, EPG])
            OH = sb("OH", [128, NT, 2, NE]); OHc = sb("OHc", [128, NE], BF16); tmpo = sb("tmpo", [128, NE])
            RK = sb("RK", [128, NT, NE]); carry = sb("carry", [128, NE])
            dg = sb("dg", [128, NT, 4]); di = sb("di", [128, NT, 2], I32)
            pad = sb("pad", [128, 4, NE]); ones = sb("ones", [128, NE])
            big = sb("big", [128, NT, NE])
            cmpb = sb("cmpb", [128, NB, NE]); be = sb("be", [128, NB])
            ixf = sb("ixf", [128, NB, KT + FT]); ixi = sb("ixi", [128, NB, KT + FT], I32)
            hb = [sb(f"hb{i}", [128, D], BF16) for i in range(2)]
            ptr = [self.ps(st, f"E_ptr{i}", [128, 512]) for i in range(2)]
            pl = self.ps(st, "E_pl", [128, 512]); pc = self.ps(st, "E_pc", [128, 512], nb=1)
            pc2 = self.ps(st, "E_pc2", [128, 512])
            self.dma("sp", cst[:, :, :], self.consts, (), cst.b)
            self.cp("dve", cstb[:, 0, :], cst[:, 1, :], cst.b, cstb.b)
            self.cp("dve", cstb[:, 1, :], cst[:, 5, :], cst.b, cstb.b)
            self.dma("sp", c2[:, :], self.consts2, (), c2.b)
            self.dma("sp", wr[:, :, :], self.wr.rearrange("(k p) n -> p k n", p=128), (), wr.b)
            self.dma("sp", brb[:, :], self.br.partition_broadcast(128), (), brb.b)
            self.memset("pool", carry[:, :], 0.0, carry.b)
            self.memset("pool", ones[:, :], 1.0, ones.b)
            for j in range(NT):
                h = hi[j % 2]
                self.dma("sp", h[:, :], self.H[j * 128:(j + 1) * 128, :], (), h.b)
                for k0 in range(0, KT, 4):
                    kn = min(4, KT - k0)
                    pt = ptr[(k0 // 4) % 2]
                    for kk in range(kn):
                        self.tr(pt[:, kk * 128:(kk + 1) * 128], h[:, (k0 + kk) * 128:(k0 + kk + 1) * 128],
                                cst[:, 0, :], h.b + cst.b, pt.b)
                    self.cp("act" if (k0 // 4) % 2 else "dve", hT[:, k0:k0 + kn, :],
                            pt[:, 0:kn * 128].rearrange("p (k t) -> p k t", k=kn), pt.b, hT.b)
                for k in range(KT):
                    self.mm(pl[:, 0:NL], hT[:, k, :], wr[:, k, :], k == 0, k == KT - 1, hT.b + wr.b, pl.b)
                self.tt("dve", lg[:, :], pl[:, 0:NL], brb[:, :], ALU.add, pl.b + brb.b, lg.b)
                self.red(sm[:, 0:1], lg[:, 0:NG], ALU.max, lg.b, sm.b)
                self.ts("dve", sm[:, 1:2], sm[:, 0:1], -1.0, None, ALU.mult, None, sm.b, sm.b)
                self.ts("dve", ohg[:, :], lg[:, 0:NG], sm[:, 0:1], None, ALU.is_equal, None, lg.b + sm.b, ohg.b)
                self.act(t3[:, 0, 0:NG] if EPG >= NG else tmpo[:, 0:NG], lg[:, 0:NG], AF.Exp, lg.b + sm.b,
                         t3.b + tmpo.b + sm.b, bias=sm[:, 1:2], accum=sm[:, 2:3])
                self.ph.op("dve", lambda e, o=sm[:, 3:4], i=sm[:, 2:3]: e.reciprocal(out=o, in_=i), sm.b, sm.b)
                lev = lg[:, NG:NL].rearrange("p (g e) -> p g e", g=NG)
                self.tt("dve", t3[:, :, :], lev, ohg[:, :].unsqueeze(2).to_broadcast([128, NG, EPG]), ALU.mult,
                        lg.b + ohg.b, t3.b)
                self.red(es[:, 0, :], t3[:, :, :].rearrange("p g e -> p e g"), ALU.add, t3.b, es.b)
                self.red(sm[:, 4:5], es[:, 0, :], ALU.max, es.b, sm.b)
                self.ts("dve", oh[:, 0, :], es[:, 0, :], sm[:, 4:5], None, ALU.is_equal, None, es.b + sm.b, oh.b)
                self.stt("dve", es[:, 1, :], oh[:, 0, :], NEG, es[:, 0, :], ALU.mult, ALU.add, oh.b + es.b, es.b)
                self.red(sm[:, 5:6], es[:, 1, :], ALU.max, es.b, sm.b)
                self.ts("dve", oh[:, 1, :], es[:, 1, :], sm[:, 5:6], None, ALU.is_equal, None, es.b + sm.b, oh.b)
                self.tt("dve", sm[:, 6:7], sm[:, 5:6], sm[:, 4:5], ALU.subtract, sm.b, sm.b)
                self.act(sm[:, 7:8], sm[:, 6:7], AF.Exp, sm.b, sm.b)
                self.ts("dve", sm[:, 8:9], sm[:, 7:8], 1.0, None, ALU.add, None, sm.b, sm.b)
                self.ph.op("dve", lambda e, o=sm[:, 9:10], i=sm[:, 8:9]: e.reciprocal(out=o, in_=i), sm.b, sm.b)
                self.tt("dve", dg[:, j, 2:3], sm[:, 9:10], sm[:, 3:4], ALU.mult, sm.b, dg.b)
                self.tt("dve", sm[:, 10:11], sm[:, 7:8], sm[:, 9:10], ALU.mult, sm.b, sm.b)
                self.tt("dve", dg[:, j, 3:4], sm[:, 10:11], sm[:, 3:4], ALU.mult, sm.b, dg.b)
                for k in range(2):
                    self.tt("dve", OH[:, j, k, :].rearrange("p (g e) -> p g e", g=NG),
                            ohg[:, :].unsqueeze(2).to_broadcast([128, NG, EPG]),
                            oh[:, k, :].unsqueeze(1).to_broadcast([128, NG, EPG]), ALU.mult, ohg.b + oh.b, OH.b)
                self.tt("dve", tmpo[:, :], OH[:, j, 0, :], OH[:, j, 1, :], ALU.add, OH.b, tmpo.b)
                self.cp("dve", OHc[:, :], tmpo[:, :], tmpo.b, OHc.b)
                self.mm(pc[:, 0:NE], cstb[:, 0, :], OHc[:, :], True, True, cstb.b + OHc.b, pc.b)
                self.mm(pc2[:, 0:NE], cstb[:, 1, :], OHc[:, :], True, True, cstb.b + OHc.b, pc2.b)
                self.tt("dve", RK[:, j, :], pc[:, 0:NE], carry[:, :], ALU.add, pc.b + carry.b, RK.b)
                self.tt("dve", carry[:, :], pc2[:, 0:NE], carry[:, :], ALU.add, pc2.b + carry.b, carry.b)
            cmp2 = cmpb[:, :, :].rearrange("p b e -> p (b e)").rearrange("p (e b) -> p e b", e=NE)
            self.tt("dve", cmp2, carry[:, :].unsqueeze(2).to_broadcast([128, NE, NB]),
                    c2[:, 0:NB].unsqueeze(1).to_broadcast([128, NE, NB]), ALU.is_gt, carry.b + c2.b, cmpb.b)
            self.red(pad[:, 0, :], cmp2, ALU.add, cmpb.b, pad.b)
            self.ts("dve", pad[:, 1, :], pad[:, 0, :], 128.0, None, ALU.mult, None, pad.b, pad.b)
            self.ph.op("dve", lambda e: e.tensor_tensor_scan(out=pad[:, 2, :], data0=ones[:, :], data1=pad[:, 1, :],
                                                            initial=0.0, op0=ALU.mult, op1=ALU.add),
                       pad.b + ones.b, pad.b)
            self.tt("dve", pad[:, 3, :], pad[:, 2, :], pad[:, 1, :], ALU.subtract, pad.b, pad.b)
            for k in range(2):
                self.tt("dve", big[:, :, :], RK[:, :, :], pad[:, 3, :].unsqueeze(1).to_broadcast([128, NT, NE]),
                        ALU.add, RK.b + pad.b, big.b)
                self.tt("dve", big[:, :, :], big[:, :, :], OH[:, :, k, :], ALU.mult, big.b + OH.b, big.b)
                self.red(dg[:, :, k], big[:, :, :], ALU.add, big.b, dg.b)
            self.cp("dve", di[:, :, :], dg[:, :, 0:2], dg.b, di.b)
            self.dma("sp", self.DEST.rearrange("(j p) f -> p j f", p=128), dg[:, :, :], dg.b, ())
            self.tt("dve", cmpb[:, :, :], pad[:, 2, :].unsqueeze(1).to_broadcast([128, NB, NE]),
                    c2[:, 0:NB].unsqueeze(2).to_broadcast([128, NB, NE]), ALU.is_le, pad.b + c2.b, cmpb.b)
            self.red(be[:, :], cmpb[:, :, :], ALU.add, cmpb.b, be.b)
            self.ts("dve", be[:, :], be[:, :], float(NE - 1), None, ALU.min, None, be.b, be.b)
            self.stt("dve", ixf[:, :, 0:KT], be[:, :].unsqueeze(2).to_broadcast([128, NB, KT]), float(D),
                     c2[:, NB:NB + KT].unsqueeze(1).to_broadcast([128, NB, KT]), ALU.mult, ALU.add,
                     be.b + c2.b, ixf.b)
            self.stt("dve", ixf[:, :, KT:KT + FT], be[:, :].unsqueeze(2).to_broadcast([128, NB, FT]), float(c.DE),
                     c2[:, NB + KT:NB + KT + FT].unsqueeze(1).to_broadcast([128, NB, FT]), ALU.mult, ALU.add,
                     be.b + c2.b, ixf.b)
            self.cp("dve", ixi[:, :, :], ixf[:, :, :], ixf.b, ixi.b)
            self.dma("sp", self.IDXS, ixi[:, :, :].rearrange("p b k -> p (b k)"), ixi.b, ())
            zb = sb("zb", [128, D], BF16)
            xbb = [Buf() for _ in range(NB)]
            self.memset("pool", zb[:, :], 0.0, zb.b)
            for b_ in range(NB):
                self.dma("sp", self.XB[b_ * 128:(b_ + 1) * 128, :], zb[:, :], zb.b, [xbb[b_]])
            for j in range(NT):
                h, hbt = hi[j % 2], hb[j % 2]
                self.dma("sp", h[:, :], self.H[j * 128:(j + 1) * 128, :], (), h.b)
                self.cp("act" if j % 2 else "dve", hbt[:, :], h[:, :], h.b, hbt.b)
                for k in range(2):
                    self.ph.op("pool", lambda e, o=self.XB, ix=di[:, j, k:k + 1], src=hbt[:, :]:
                               e.indirect_dma_start(out=o, out_offset=bass.IndirectOffsetOnAxis(ap=ix, axis=0),
                                                    in_=src, in_offset=None),
                               hbt.b + di.b, xbb, dma=True)
            ph.emit()

    def phase_experts(self):
        c, nc = self.c, self.nc
        D, KT, NB, DE = c.D, c.KT, c.NB, c.DE
        FT = DE // 128
        with contextlib.ExitStack() as st:
            self.ph = ph = Phase(nc, "F")
            sb = lambda n, s, dt=F32, nb=1: self.sb(st, "F_" + n, s, dt, nb)
            cst = sb("cst", [128, 128]); identb = sb("idb", [128, 128], BF16)
            ixi = sb("ixi", [128, NB, KT + FT], I32)
            xb = [sb(f"xb{i}", [128, D], BF16) for i in range(2)]
            xT = sb("xT", [128, KT, 128], BF16)
            wg = [sb(f"wg{i}", [128, KT, DE], BF16) for i in range(2)]
            wu = [sb(f"wu{i}", [128, KT, DE], BF16) for i in range(2)]
            wd = [sb(f"wd{i}", [128, FT, D], BF16) for i in range(2)]
            sg = sb("sg", [128, DE]); hid = sb("hid", [128, DE], BF16); hT = sb("hT", [128, FT, 128], BF16)
            yb = [sb(f"yb{i}", [128, D]) for i in range(2)]
            ptr = self.ps(st, "F_ptr", [128, 1024], BF16)
            pg = self.ps(st, "F_pg", [128, 512]); pu = self.ps(st, "F_pu", [128, 512])
            py = self.ps(st, "F_py", [128, D])
            self.dma("sp", cst[:, :], self.consts[:, 0, :], (), cst.b)
            self.cp("dve", identb[:, :], cst[:, :], cst.b, identb.b)
            self.dma("sp", ixi[:, :, :].rearrange("p b k -> p (b k)"), self.IDXS, (), ixi.b)

            def gather(dst, src, ix, reads, writes):
                self.ph.op("pool", lambda e: e.indirect_dma_start(
                    out=dst, out_offset=None, in_=src, in_offset=bass.IndirectOffsetOnAxis(ap=ix, axis=0)),
                    reads, writes, dma=True)

            for b in range(NB):
                x, g, u, dn, y = xb[b % 2], wg[b % 2], wu[b % 2], wd[b % 2], yb[b % 2]
                self.dma("sp", x[:, :], self.XB[b * 128:(b + 1) * 128, :], (), x.b)
                for k in range(KT):
                    gather(g[:, k, :], self.w_gate, ixi[:, b, k:k + 1], ixi.b, g.b)
                    gather(u[:, k, :], self.w_up, ixi[:, b, k:k + 1], ixi.b, u.b)
                for f in range(FT):
                    gather(dn[:, f, :], self.w_down, ixi[:, b, KT + f:KT + f + 1], ixi.b, dn.b)
                for k0 in range(0, KT, 8):
                    kn = min(8, KT - k0)
                    for kk in range(kn):
                        self.tr(ptr[:, kk * 128:(kk + 1) * 128], x[:, (k0 + kk) * 128:(k0 + kk + 1) * 128],
                                identb[:, :], x.b + identb.b, ptr.b)
                    self.cp("act", xT[:, k0:k0 + kn, :], ptr[:, 0:kn * 128].rearrange("p (k t) -> p k t", k=kn),
                            ptr.b, xT.b)
                for k in range(KT):
                    self.mm(pg[:, 0:DE], xT[:, k, :], g[:, k, :], k == 0, k == KT - 1, xT.b + g.b, pg.b)
                for k in range(KT):
                    self.mm(pu[:, 0:DE], xT[:, k, :], u[:, k, :], k == 0, k == KT - 1, xT.b + u.b, pu.b)
                self.act(sg[:, :], pg[:, 0:DE], AF.Silu, pg.b, sg.b)
                self.tt("dve", hid[:, :], pu[:, 0:DE], sg[:, :], ALU.mult, sg.b + pu.b, hid.b)
                for f in range(FT):
                    self.tr(ptr[:, f * 128:(f + 1) * 128], hid[:, f * 128:(f + 1) * 128], identb[:, :],
                            hid.b + identb.b, ptr.b)
                self.cp("act", hT[:, :, :], ptr[:, 0:FT * 128].rearrange("p (k t) -> p k t", k=FT), ptr.b, hT.b)
                for n0 in range(0, D, 512):
                    nn = min(512, D - n0)
                    for f in range(FT):
                        self.mm(py[:, n0:n0 + nn], hT[:, f, :], dn[:, f, n0:n0 + nn], f == 0, f == FT - 1,
                                hT.b + dn.b, py.b)
                self.cp("dve", y[:, :], py[:, :], py.b, y.b)
                self.dma("sp", self.YB[b * 128:(b + 1) * 128, :], y[:, :], y.b, ())
            ph.emit()

    def phase_final(self):
        c, nc = self.c, self.nc
        D, NT = c.D, c.NT
        with contextlib.ExitStack() as st:
            self.ph = ph = Phase(nc, "G")
            sb = lambda n, s, dt=F32, nb=1: self.sb(st, "G_" + n, s, dt, nb)
            gb = sb("gb", [128, D]); bb = sb("bb", [128, D])
            dg = sb("dg", [128, NT, 4]); di = sb("di", [128, NT, 2], I32)
            hi = [sb(f"hi{i}", [128, D]) for i in range(2)]
            y0 = [sb(f"y0{i}", [128, D]) for i in range(2)]
            y1 = [sb(f"y1{i}", [128, D]) for i in range(2)]
            res = sb("res", [128, D]); out = [sb(f"out{i}", [128, D]) for i in range(2)]
            scr = sb("scr", [128, D]); sm = sb("sm", [128, 8])
            self.dma("sp", gb[:, :], self.ln2_g.partition_broadcast(128), (), gb.b)
            self.dma("sp", bb[:, :], self.ln2_b.partition_broadcast(128), (), bb.b)
            self.dma("sp", dg[:, :, :], self.DEST.rearrange("(j p) f -> p j f", p=128), (), dg.b)
            self.cp("dve", di[:, :, :], dg[:, :, 0:2], dg.b, di.b)
            for j in range(NT):
                h, a0, a1, o = hi[j % 2], y0[j % 2], y1[j % 2], out[j % 2]
                self.dma("sp", h[:, :], self.H[j * 128:(j + 1) * 128, :], (), h.b)
                for k, a in ((0, a0), (1, a1)):
                    self.ph.op("pool", lambda e, dst=a[:, :], ix=di[:, j, k:k + 1]: e.indirect_dma_start(
                        out=dst, out_offset=None, in_=self.YB, in_offset=bass.IndirectOffsetOnAxis(ap=ix, axis=0)),
                        di.b, a.b, dma=True)
                self.ts("dve", res[:, :], h[:, :], c.alpha, None, ALU.mult, None, h.b, res.b)
                self.stt("dve", res[:, :], a0[:, :], dg[:, j, 2:3], res[:, :], ALU.mult, ALU.add,
                         a0.b + dg.b + res.b, res.b)
                self.stt("dve", res[:, :], a1[:, :], dg[:, j, 3:4], res[:, :], ALU.mult, ALU.add,
                         a1.b + dg.b + res.b, res.b)
                self.layer_norm(res, o, gb, bb, sm, scr, D)
                self.dma("sp", self.y[j * 128:(j + 1) * 128, :], o[:, :], o.b, ())
            ph.emit()

    def build(self):
        self.declare()
        self.phase_proj()
        self.phase_attn()
        self.phase_rwkv(0)
        self.phase_rwkv(1)
        self.phase_out()
        self.phase_route()
        self.phase_experts()
        self.phase_final()
        return self.nc


def t5_bucket_np(rel):
    half, max_exact = 16, 8
    ret = np.where(rel > 0, half, 0)
    n = np.abs(rel)
    nf = np.maximum(n, 1).astype(np.float32)
    large = max_exact + (np.log(nf / max_exact) / math.log(128 / max_exact) * (half - max_exact)).astype(np.int32)
    large = np.minimum(large, half - 1)
    return ret + np.where(n < max_exact, n, large)


def static_consts():
    i = np.arange(128)[:, None]
    t = np.arange(128)[None, :]
    cst = np.zeros((128, 8, 128), np.float32)
    cst[:, 0] = (i == t)
    cst[:, 1] = (i < t)
    cst[:, 2] = (i <= t)
    cst[:, 3] = (i > t)
    cst[:, 4] = (i >= t)
    cst[:, 5] = 1.0
    qi = np.arange(128)[:, None]
    kj = np.arange(384)[None, :]
    rel = kj - 128 - qi
    bim = np.zeros((128, 2, 384), np.float32)
    bim[:, 0] = t5_bucket_np(rel)
    bim[:, 1] = np.where(np.abs(rel) <= 128, 0.0, NEG)
    return cst, bim


def core_flags(seq_tiles, NT):
    hp = np.zeros(NT, np.float32)
    hn = np.zeros(NT, np.float32)
    j = 0
    for n in seq_tiles:
        for q in range(n):
            hp[j + q] = 1.0 if q > 0 else 0.0
            hn[j + q] = 1.0 if q < n - 1 else 0.0
        j += n
    f = np.zeros((128, 6, NT), np.float32)
    f[:, 0] = 1.0
    f[0, 0] = hp
    f[:, 1] = 1.0
    f[127, 1] = hn
    f[:, 2] = (hp - 1.0) * 1.0e30
    f[:, 3] = (hn - 1.0) * 1.0e30
    f[:, 4] = hp
    f[:, 5] = hn
    return f


def shared_inputs(cfg, inp):
    c = cfg
    cst, bim = static_consts()
    sq = lambda a: np.ascontiguousarray(np.asarray(a)[0])
    c2 = np.zeros((128, c.NB + c.KT + c.DE // 128), np.float32)
    c2[:, 0:c.NB] = (np.arange(c.NB) * 128.0)[None, :]
    c2[:, c.NB:] = np.arange(c.KT + c.DE // 128)[None, :] * 128.0 + np.arange(128)[:, None]
    c2[:, c.NB + c.KT:] = np.arange(c.DE // 128)[None, :] * 128.0 + np.arange(128)[:, None]
    d = {
        "consts": cst, "bimat": bim, "consts2": c2,
        "rel_bias": np.ascontiguousarray(np.asarray(inp["rel_bias"]).reshape(-1)),
        "w_in": sq(inp["w_in"]), "attn_sink": sq(inp["attn_sink"]), "shift_mu": sq(inp["shift_mu"]),
        "decay_w0": sq(inp["decay_w0"]), "decay_w2": sq(inp["decay_w2"]),
        "iclr_a0": sq(inp["iclr_a0"]), "iclr_a2": sq(inp["iclr_a2"]), "gate_g2": sq(inp["gate_g2"]),
        "k_k": sq(inp["k_k"]), "k_a": sq(inp["k_a"]), "r_k": sq(inp["r_k"]).reshape(-1),
        "gn_g": sq(inp["gn_g"]), "gn_b": sq(inp["gn_b"]), "w_out": sq(inp["w_out"]),
        "ln1_g": sq(inp["ln1_g"]), "ln1_b": sq(inp["ln1_b"]),
        "wr": np.ascontiguousarray(np.concatenate([sq(inp["router_group_w"]), sq(inp["router_expert_w"])], axis=1)),
        "br": np.ascontiguousarray(np.concatenate([sq(inp["router_group_b"]), sq(inp["router_expert_b"])], axis=0)),
        "w_gate": sq(inp["w_gate"]).reshape(c.NE * c.D, c.DE),
        "w_up": sq(inp["w_up"]).reshape(c.NE * c.D, c.DE),
        "w_down": sq(inp["w_down"]).reshape(c.NE * c.DE, c.D),
        "ln2_g": sq(inp["ln2_g"]), "ln2_b": sq(inp["ln2_b"]),
    }
    for k in BIGW:
        d[k] = np.concatenate([d[k], np.zeros((1, d[k].shape[1]), np.float32)], axis=0)
    return d


BIGW = ("w_in", "w_out", "w_gate", "w_up", "w_down")


def assign(xp, xs, n_cores):
    seqs = [("p", b, xp.shape[1] // 128) for b in range(xp.shape[0])] + \
           [("s", b, xs.shape[1] // 128) for b in range(xs.shape[0])]
    total = sum(s[2] for s in seqs)
    per = total // n_cores
    cores = [[] for _ in range(n_cores)]
    load = [0] * n_cores
    for s in sorted(seqs, key=lambda s: -s[2]):
        for ci in range(n_cores):
            if load[ci] + s[2] <= per:
                cores[ci].append(s)
                load[ci] += s[2]
                break
        else:
            raise ValueError("cannot balance sequences over cores")
    assert all(l == per for l in load)
    return cores, per


def kernel(**inputs):
    n_cores = 8
    cfg = Cfg()
    xp = np.asarray(inputs["x_prompt"], dtype=np.float32)
    xs = np.asarray(inputs["x_sample"], dtype=np.float32)
    cores, per = assign(xp, xs, n_cores)
    assert per == cfg.NT
    nc = Builder(cfg).build()
    sh = shared_inputs(cfg, inputs)
    in_maps = []
    for ci, segs in enumerate(cores):
        m = dict(sh)
        if ci > 0:
            for k in BIGW:
                m[k] = sh[k].copy()
                m[k][-1, :] = float(ci)
        m["x"] = np.ascontiguousarray(np.concatenate([(xp if s[0] == "p" else xs)[s[1]] for s in segs], axis=0))
        m["flags"] = core_flags([s[2] for s in segs], cfg.NT)
        in_maps.append(m)
    res = run_bass_kernel_spmd(nc, in_maps, core_ids=list(range(n_cores)))
    yp = np.zeros(xp.shape, np.float32)
    ys = np.zeros(xs.shape, np.float32)
    for ci, segs in enumerate(cores):
        y = np.asarray(res.results[ci]["y"])
        r0 = 0
        for s in segs:
            n = s[2] * 128
            (yp if s[0] == "p" else ys)[s[1]] = y[r0:r0 + n]
            r0 += n
    return (yp, ys)
```
